# Optimizing a Trainium2 kernel written in Bass

```python
import math
import jax
import jax.numpy as jnp
from jax import lax
import numpy as np

D_MODEL = 2048
BATCH = 2
SEQ = 8192
DEPTH = 1

N_META = 16
BLOCK = 128
PAD = BLOCK - N_META
WINDOW = 128
HQ = 16
HKV = 4
Q_PER_KV = HQ // HKV
HD = 64
D_INNER = 2048
SSD_HEADDIM = 64
SSD_HEADS = D_INNER // SSD_HEADDIM
SSD_GROUPS = 4
HEADS_PER_GROUP = SSD_HEADS // SSD_GROUPS
D_STATE = 128
CONV_W = 4
CONV_DIM = D_INNER + 2 * SSD_GROUPS * D_STATE
MOE_GROUPS = 8
EXPERTS_PER_GROUP = 8
N_EXPERTS = MOE_GROUPS * EXPERTS_PER_GROUP
TOP_K = 2
D_EXPERT = 512
MOE_BLOCK = 128
LN_EPS = 1e-5
RMS_EPS = 1e-5
NEG_INF = -1e30
ALPHA = (2.0 * DEPTH) ** 0.25
BETA = (8.0 * DEPTH) ** -0.25
IN_SIZES = (HQ * HD, HKV * HD, HKV * HD, D_INNER, CONV_DIM, SSD_HEADS, D_MODEL, D_MODEL)
IN_DIM = sum(IN_SIZES)
IN_SPLITS = tuple(int(s) for s in np.cumsum(IN_SIZES)[:-1])

kernel_name = 'hybrid_swa_ssd_hmoe_deepnorm'


def layer_norm(x, g, b):
    xf = x.astype(jnp.float32)
    mu = jnp.mean(xf, axis=-1, keepdims=True)
    var = jnp.mean(jnp.square(xf - mu), axis=-1, keepdims=True)
    y = (xf - mu) * lax.rsqrt(var + LN_EPS) * g.astype(jnp.float32) + b.astype(jnp.float32)
    return y.astype(x.dtype)


def pad_front(a):
    return jnp.pad(a, [(0, 0), (PAD, 0)] + [(0, 0)] * (a.ndim - 2))


def sliding_window_attention_with_sinks(q, k, v, sinks):
    b, lp = q.shape[0], q.shape[1]
    nb = lp // BLOCK
    qb = q.reshape(b, nb, BLOCK, HKV, Q_PER_KV, HD)
    kb = k.reshape(b, nb, BLOCK, HKV, HD)
    vb = v.reshape(b, nb, BLOCK, HKV, HD)

    def band(t):
        prev = jnp.concatenate([jnp.zeros_like(t[:, :1]), t[:, :-1]], axis=1)
        return jnp.concatenate([prev, t], axis=2)

    k_band, v_band = band(kb), band(vb)
    k_meta = k[:, PAD:PAD + N_META]
    v_meta = v[:, PAD:PAD + N_META]
    scale = HD ** -0.5
    s_band = jnp.einsum('bnqhgd,bnshd->bnhgqs', qb, k_band).astype(jnp.float32) * scale
    s_meta = jnp.einsum('bnqhgd,bmhd->bnhgqm', qb, k_meta).astype(jnp.float32) * scale
    blk = jnp.arange(nb)[:, None]
    q_pos = blk * BLOCK + jnp.arange(BLOCK)[None, :] - PAD
    k_pos = (blk - 1) * BLOCK + jnp.arange(2 * BLOCK)[None, :] - PAD
    dist = q_pos[:, :, None] - k_pos[:, None, :]
    band_ok = (k_pos[:, None, :] >= N_META) & (dist >= 0) & (dist < WINDOW)
    meta_ok = jnp.arange(N_META)[None, None, :] <= q_pos[:, :, None]
    s_band = jnp.where(band_ok[None, :, None, None], s_band, NEG_INF)
    s_meta = jnp.where(meta_ok[None, :, None, None], s_meta, NEG_INF)
    sink = jnp.broadcast_to(sinks.astype(jnp.float32).reshape(1, 1, HKV, Q_PER_KV, 1, 1),
                            s_band.shape[:-1] + (1,))
    probs = jax.nn.softmax(jnp.concatenate([s_band, s_meta, sink], axis=-1), axis=-1).astype(v.dtype)
    out = (jnp.einsum('bnhgqs,bnshd->bnqhgd', probs[..., :2 * BLOCK], v_band)
           + jnp.einsum('bnhgqm,bmhd->bnqhgd', probs[..., 2 * BLOCK:2 * BLOCK + N_META], v_meta))
    return out.reshape(b, lp, HQ * HD)


def causal_depthwise_conv(x, w, bias):
    y = lax.conv_general_dilated(x, w[:, None, :], window_strides=(1,), padding=[(CONV_W - 1, 0)],
                                 dimension_numbers=('NWC', 'WIO', 'NWC'),
                                 feature_group_count=x.shape[-1])
    return y + bias


def ssd_chunked(xh, dt, a, bm, cm):
    b, lp = xh.shape[0], xh.shape[1]
    nc = lp // BLOCK
    xdt = (xh * dt[..., None]).reshape(b, nc, BLOCK, SSD_GROUPS, HEADS_PER_GROUP, SSD_HEADDIM)
    adt = (dt * a).reshape(b, nc, BLOCK, SSD_GROUPS, HEADS_PER_GROUP).transpose(0, 3, 4, 1, 2)
    a_cs = jnp.cumsum(adt, axis=-1)
    bc = bm.reshape(b, nc, BLOCK, SSD_GROUPS, D_STATE)
    cc = cm.reshape(b, nc, BLOCK, SSD_GROUPS, D_STATE)
    causal = jnp.tril(jnp.ones((BLOCK, BLOCK), dtype=bool))
    seg = a_cs[..., :, None] - a_cs[..., None, :]
    decay_ls = jnp.exp(jnp.where(causal, seg, -jnp.inf))
    cb = jnp.einsum('bclgn,bcsgn->bcgls', cc, bc)
    y_diag = jnp.einsum('bcgls,bgrcls,bcsgrp->bclgrp', cb, decay_ls, xdt)
    decay_to_end = jnp.exp(a_cs[..., -1:] - a_cs)
    states = jnp.einsum('bclgn,bgrcl,bclgrp->bcgrpn', bc, decay_to_end, xdt)
    chunk_decay = jnp.exp(a_cs[..., -1])

    def step(h, inp):
        s_c, d_c = inp
        return h * d_c[..., None, None] + s_c, h

    h0 = jnp.zeros((b, SSD_GROUPS, HEADS_PER_GROUP, SSD_HEADDIM, D_STATE), xh.dtype)
    _, prev = lax.scan(step, h0, (jnp.moveaxis(states, 1, 0), jnp.moveaxis(chunk_decay, -1, 0)))
    prev = jnp.moveaxis(prev, 0, 1)
    y_off = jnp.einsum('bclgn,bcgrpn,bgrcl->bclgrp', cc, prev, jnp.exp(a_cs))
    return (y_diag + y_off).reshape(b, lp, SSD_HEADS, SSD_HEADDIM)


def gated_rmsnorm(y, z, g):
    b, l, _ = y.shape
    yz = (y * jax.nn.silu(z.astype(jnp.float32))).reshape(b, l, SSD_GROUPS, D_INNER // SSD_GROUPS)
    yz = yz * lax.rsqrt(jnp.mean(jnp.square(yz), axis=-1, keepdims=True) + RMS_EPS)
    return yz.reshape(b, l, D_INNER) * g.astype(jnp.float32)


def hybrid_mixer(u, w_in, conv_w, conv_b, dt_bias, a_log, d_skip, ssd_norm_g, sinks,
                 w_br_attn, w_br_ssd, w_o):
    b, l, _ = u.shape
    q, k, v, z, xbc, dt_raw, g_attn, g_ssd = jnp.split(u @ w_in, IN_SPLITS, axis=-1)
    q = pad_front(q.reshape(b, l, HKV, Q_PER_KV, HD))
    k = pad_front(k.reshape(b, l, HKV, HD))
    v = pad_front(v.reshape(b, l, HKV, HD))
    attn = sliding_window_attention_with_sinks(q, k, v, sinks)[:, PAD:]
    xbc = jax.nn.silu(causal_depthwise_conv(xbc, conv_w, conv_b))
    xs, bs, cs = jnp.split(xbc, [D_INNER, D_INNER + SSD_GROUPS * D_STATE], axis=-1)
    dt = jax.nn.softplus(dt_raw.astype(jnp.float32) + dt_bias.astype(jnp.float32))
    a = -jnp.exp(a_log.astype(jnp.float32))
    xh = xs.reshape(b, l, SSD_HEADS, SSD_HEADDIM).astype(jnp.float32)
    y = ssd_chunked(pad_front(xh), pad_front(dt), a,
                    pad_front(bs.reshape(b, l, SSD_GROUPS, D_STATE).astype(jnp.float32)),
                    pad_front(cs.reshape(b, l, SSD_GROUPS, D_STATE).astype(jnp.float32)))[:, PAD:]
    y = y + d_skip.astype(jnp.float32)[:, None] * xh
    y = gated_rmsnorm(y.reshape(b, l, D_INNER), z, ssd_norm_g).astype(u.dtype)
    merged = jax.nn.sigmoid(g_attn) * (attn @ w_br_attn) + jax.nn.sigmoid(g_ssd) * (y @ w_br_ssd)
    return merged @ w_o


def hierarchical_moe(h, w_rg, b_rg, w_re, b_re, w_gate, w_up, w_down):
    b, l, d = h.shape
    t = b * l
    xt = h.reshape(t, d)
    p_group = jax.nn.softmax((xt @ w_rg).astype(jnp.float32) + b_rg.astype(jnp.float32), axis=-1)
    p_top, g_idx = lax.top_k(p_group, 1)
    le = ((xt @ w_re).astype(jnp.float32) + b_re.astype(jnp.float32)).reshape(t, MOE_GROUPS, EXPERTS_PER_GROUP)
    le_sel = jnp.take_along_axis(le, g_idx[:, :, None], axis=1)[:, 0]
    w_in_grp, e_in_grp = lax.top_k(jax.nn.softmax(le_sel, axis=-1), TOP_K)
    w_in_grp = w_in_grp / jnp.sum(w_in_grp, axis=-1, keepdims=True)
    weight = (p_top * w_in_grp).astype(h.dtype)
    expert = g_idx * EXPERTS_PER_GROUP + e_in_grp
    n = t * TOP_K
    nb = -(-n // MOE_BLOCK) + N_EXPERTS
    e_flat = expert.reshape(n)
    w_flat = weight.reshape(n)
    tok_flat = jnp.arange(n, dtype=jnp.int32) // TOP_K
    order = jnp.argsort(e_flat, stable=True)
    e_sorted = e_flat[order]
    counts = jnp.zeros((N_EXPERTS,), jnp.int32).at[e_flat].add(1)
    start = jnp.cumsum(counts) - counts
    pcounts = (counts + MOE_BLOCK - 1) // MOE_BLOCK * MOE_BLOCK
    pend = jnp.cumsum(pcounts)
    pstart = pend - pcounts
    dest = pstart[e_sorted] + jnp.arange(n, dtype=jnp.int32) - start[e_sorted]
    buf_tok = jnp.full((nb * MOE_BLOCK,), t, jnp.int32).at[dest].set(tok_flat[order])
    buf_w = jnp.zeros((nb * MOE_BLOCK,), h.dtype).at[dest].set(w_flat[order])
    blk_e = jnp.minimum(jnp.searchsorted(pend, jnp.arange(nb, dtype=jnp.int32) * MOE_BLOCK, side='right'),
                        N_EXPERTS - 1).astype(jnp.int32)
    x_pad = jnp.concatenate([xt, jnp.zeros((1, d), xt.dtype)], axis=0)
    xb = x_pad[buf_tok].reshape(nb, MOE_BLOCK, d)

    def expert_block(args):
        xblk, e = args
        hid = jax.nn.silu(xblk @ w_gate[e]) * (xblk @ w_up[e])
        return hid @ w_down[e]

    yb = lax.map(expert_block, (xb, blk_e)).reshape(nb * MOE_BLOCK, d) * buf_w[:, None]
    out = jnp.zeros((t + 1, d), h.dtype).at[buf_tok].add(yb)[:t]
    return out.reshape(b, l, d)


def setup_inputs(seed: int = 0) -> dict:
    key = jax.random.key(seed)
    ks = jax.random.split(key, 26)
    f32 = jnp.float32

    def nrm(k, shape, scale):
        return jax.random.normal(k, shape, f32) * scale

    dt = jnp.exp(jax.random.uniform(ks[7], (DEPTH, SSD_HEADS), f32, math.log(1e-3), math.log(1e-1)))
    return {
        'x': nrm(ks[0], (BATCH, SEQ, D_MODEL), 1.0),
        'meta_tokens': nrm(ks[1], (N_META, D_MODEL), 1.0),
        'ln_emb_g': 1.0 + nrm(ks[2], (D_MODEL,), 0.02),
        'ln_emb_b': nrm(ks[3], (D_MODEL,), 0.02),
        'w_in': nrm(ks[4], (DEPTH, D_MODEL, IN_DIM), D_MODEL ** -0.5),
        'conv_w': nrm(ks[5], (DEPTH, CONV_W, CONV_DIM), CONV_W ** -0.5),
        'conv_b': nrm(ks[6], (DEPTH, CONV_DIM), 0.01),
        'dt_bias': dt + jnp.log(-jnp.expm1(-dt)),
        'a_log': jnp.log(jax.random.uniform(ks[8], (DEPTH, SSD_HEADS), f32, 1.0, 16.0)),
        'd_skip': 1.0 + nrm(ks[9], (DEPTH, SSD_HEADS), 0.02),
        'ssd_norm_g': 1.0 + nrm(ks[10], (DEPTH, D_INNER), 0.02),
        'sinks': nrm(ks[11], (DEPTH, HQ), 0.5),
        'w_br_attn': nrm(ks[12], (DEPTH, HQ * HD, D_MODEL), (HQ * HD) ** -0.5),
        'w_br_ssd': nrm(ks[13], (DEPTH, D_INNER, D_MODEL), D_INNER ** -0.5),
        'w_o': nrm(ks[14], (DEPTH, D_MODEL, D_MODEL), BETA * D_MODEL ** -0.5),
        'ln1_g': 1.0 + nrm(ks[15], (DEPTH, D_MODEL), 0.02),
        'ln1_b': nrm(ks[16], (DEPTH, D_MODEL), 0.02),
        'w_router_group': nrm(ks[17], (DEPTH, D_MODEL, MOE_GROUPS), D_MODEL ** -0.5),
        'b_router_group': nrm(ks[18], (DEPTH, MOE_GROUPS), 0.01),
        'w_router_expert': nrm(ks[19], (DEPTH, D_MODEL, N_EXPERTS), D_MODEL ** -0.5),
        'b_router_expert': nrm(ks[20], (DEPTH, N_EXPERTS), 0.01),
        'w_gate': nrm(ks[21], (DEPTH, N_EXPERTS, D_MODEL, D_EXPERT), D_MODEL ** -0.5),
        'w_up': nrm(ks[22], (DEPTH, N_EXPERTS, D_MODEL, D_EXPERT), D_MODEL ** -0.5),
        'w_down': nrm(ks[23], (DEPTH, N_EXPERTS, D_EXPERT, D_MODEL), BETA * D_EXPERT ** -0.5),
        'ln2_g': 1.0 + nrm(ks[24], (DEPTH, D_MODEL), 0.02),
        'ln2_b': nrm(ks[25], (DEPTH, D_MODEL), 0.02),
    }


def reference(x, meta_tokens, ln_emb_g, ln_emb_b, w_in, conv_w, conv_b, dt_bias, a_log, d_skip,
              ssd_norm_g, sinks, w_br_attn, w_br_ssd, w_o, ln1_g, ln1_b, w_router_group,
              b_router_group, w_router_expert, b_router_expert, w_gate, w_up, w_down, ln2_g, ln2_b):
    b = x.shape[0]
    meta = jnp.broadcast_to(meta_tokens.astype(x.dtype)[None], (b, N_META, x.shape[-1]))
    h = layer_norm(jnp.concatenate([meta, x], axis=1), ln_emb_g, ln_emb_b)
    for i in range(DEPTH):
        mix = hybrid_mixer(h, w_in[i], conv_w[i], conv_b[i], dt_bias[i], a_log[i], d_skip[i],
                           ssd_norm_g[i], sinks[i], w_br_attn[i], w_br_ssd[i], w_o[i])
        h = layer_norm(ALPHA * h + mix, ln1_g[i], ln1_b[i])
        ffn = hierarchical_moe(h, w_router_group[i], b_router_group[i], w_router_expert[i],
                               b_router_expert[i], w_gate[i], w_up[i], w_down[i])
        h = layer_norm(ALPHA * h + ffn, ln2_g[i], ln2_b[i])
    return h[:, N_META:]
```

```python
import numpy as np
from contextlib import ExitStack
import concourse.bass as bass
import concourse.mybir as mybir
from concourse.bass_utils import run_bass_kernel_spmd

F32 = mybir.dt.float32
BF16 = mybir.dt.bfloat16
I32 = mybir.dt.int32
AF = mybir.ActivationFunctionType
ALU = mybir.AluOpType
AX = mybir.AxisListType

D = 2048
KC = 16
NA_CH = 49
SUB_CH = 7
NSUB = 7
NB_CH = 18
OWN = 2048
IN_DIM = 10784
OFF_Q, OFF_K, OFF_V, OFF_Z, OFF_X, OFF_B, OFF_C, OFF_DT, OFF_GA, OFF_GS = (
    0, 1024, 1280, 1536, 3584, 5632, 6144, 6656, 6688, 8736)
ALPHA = 2.0 ** 0.25
LN_EPS = 1e-5
RMS_EPS = 1e-5
NSLOT = 64 * 128


class Sync:
    def __init__(self, nc, es, n_dma_sems=24):
        self.nc = nc
        self.eng = {"pe": nc.tensor, "dve": nc.vector, "act": nc.scalar, "pool": nc.gpsimd, "sp": nc.sync}
        self.sem = {}
        self.cnt = {}
        for e in self.eng:
            self.sem["s_" + e] = es.enter_context(nc.semaphore("s_" + e))
            self.cnt["s_" + e] = 0
        self.dsems = []
        for i in range(n_dma_sems):
            n = "d%d" % i
            self.sem[n] = es.enter_context(nc.semaphore(n))
            self.cnt[n] = 0
            self.dsems.append(n)
        self.sem["cc"] = es.enter_context(nc.semaphore("cc"))
        self.cnt["cc"] = 0
        self.rr = 0
        self.seen = {e: {} for e in self.eng}
        self.bufs = {}

    def _wait(self, e, s, v):
        if v <= 0 or self.seen[e].get(s, 0) >= v:
            return
        self.eng[e].wait_ge(self.sem[s], v)
        self.seen[e][s] = v

    def _deps(self, reads, writes, me=None):
        deps = {}

        def add(d, skip=None):
            for s, v in d.items():
                if s == skip:
                    continue
                if deps.get(s, 0) < v:
                    deps[s] = v
        for r in reads:
            b = self.bufs.get(r)
            if b:
                add(b["w"])
                if isinstance(r, tuple) and r[0] == "ps":
                    add(b["r"], skip=me)
        for w in writes:
            b = self.bufs.get(w)
            if b:
                add(b["w"])
                add(b["r"])
        return deps

    def _record(self, reads, writes, tok):
        s, v = tok
        for r in reads:
            b = self.bufs.setdefault(r, {"w": {}, "r": {}})
            b["r"][s] = v
        for w in writes:
            self.bufs[w] = {"w": {s: v}, "r": {}}

    def op(self, e, fn, reads=(), writes=()):
        me = "s_" + e
        deps = self._deps(reads, writes, me)
        for s, v in deps.items():
            if e == "pe" and s == me:
                continue
            self._wait(e, s, v)
        ins = fn()
        self.cnt[me] += 1
        ins.then_inc(self.sem[me], 1)
        self._record(reads, writes, (me, self.cnt[me]))

    def dma(self, q, fn, reads=(), writes=()):
        s = self.dsems[self.rr]
        self.rr = (self.rr + 1) % len(self.dsems)
        self._wait(q, s, self.cnt[s])
        for ds, v in self._deps(reads, writes).items():
            self._wait(q, ds, v)
        ins = fn(self.eng[q])
        self.cnt[s] += 16
        ins.then_inc(self.sem[s], 16)
        self._record(reads, writes, (s, self.cnt[s]))

    def collective(self, fn, reads=(), writes=()):
        if "cc" not in self.sem:
            raise RuntimeError("no cc sem")
        for ds, v in self._deps(reads, writes).items():
            self._wait("pool", ds, v)
        ins = fn()
        self.cnt["cc"] += 1
        ins.then_inc(self.sem["cc"])
        self._record(reads, writes, ("cc", self.cnt["cc"]))

    def barrier(self):
        for e in self.eng:
            for s, v in self.cnt.items():
                self._wait(e, s, v)
        self.bufs = {}


def build(dbg=(), stop_after=None, nocc=False):
    nc = bass.Bass("TRN2", target_bir_lowering=False)
    es = ExitStack()
    S = Sync(nc, es)

    def din(name, shape, dt=F32):
        return nc.dram_tensor(name, list(shape), dt, kind="ExternalInput").ap()

    def dscr(name, shape, dt):
        kind = "ExternalOutput" if name in dbg else "Internal"
        return nc.dram_tensor(name, list(shape), dt, kind=kind).ap()

    def sb(name, shape, dt, stack=None):
        return (stack or es).enter_context(nc.sbuf_tensor(name, list(shape), dt))

    xb = din("xb", [NB_CH * 128, D])
    hmask = din("hmask", [128, 128 + 1 + 8])
    consts = din("consts", [128, 8 * 128])
    mfirst = din("mfirst", [128, 2 * 128])
    lnp = din("lnp", [128, 6 * D])
    gnorm = din("gnorm", [128, D])
    w_in = din("w_in", [D, IN_DIM])
    convw = din("convw", [128, 24 * 4])
    convb = din("convb", [128, 24])
    small = din("small", [128, 32 * 3 + 16 + 72 + 64])
    w_bra = din("w_bra", [1024, D])
    w_brs = din("w_brs", [D, D])
    w_o = din("w_o", [D, D])
    w_r = din("w_r", [D, 72])
    w_gate = din("w_gate", [64, D, 512])
    w_up = din("w_up", [64, D, 512])
    w_down = din("w_down", [64, 512, D])
    out = nc.dram_tensor("out", [OWN, D], F32, kind="ExternalOutput").ap()

    U = dscr("U", [OWN, D], F32)
    Q_T = dscr("Q_T", [1024, OWN], BF16)
    K_T = dscr("K_T", [256, NB_CH * 128], BF16)
    V_ = dscr("V_", [NB_CH * 128, 256], BF16)
    Z_ = dscr("Z_", [OWN, D], BF16)
    XS = dscr("XS", [17 * 128, 2560], BF16)
    GSRC = dscr("GSRC", [128, 2080], F32)
    GDST = dscr("GDST", [8 * 128, 2080], F32)
    BC_T = dscr("BC_T", [1024, OWN], BF16)
    DT_ = dscr("DT_", [17 * 128, 32], F32)
    G_T = dscr("G_T", [4096, OWN], BF16)
    YN_T = dscr("YN_T", [D, OWN], BF16)
    AT_T = dscr("AT_T", [1024, OWN], BF16)
    M_T = dscr("M_T", [D, OWN], BF16)
    H1 = dscr("H1", [OWN, D], F32)
    XM = dscr("XM", [NSLOT, D], BF16)
    YS = dscr("YS", [NSLOT, D], F32)
    HDBG = dscr("HDBG", [128, D], F32)

    cst = sb("cst", [128, 8 * 128], F32)
    ident_f = cst[:, 0:128]
    tri_le = cst[:, 128:256]
    triU = cst[:, 256:384]
    ones_f = cst[:, 384:512]
    maskneg = cst[:, 512:640]
    triS = cst[:, 640:768]
    mcur_f = cst[:, 768:896]
    mprev_f = cst[:, 896:1024]
    ident_b = sb("ident_b", [128, 128], BF16)
    ones_b = sb("ones_b", [128, 128], BF16)
    sm = sb("sm", [128, 32 * 3 + 16 + 72 + 64], F32)
    dtb_bc = sm[:, 0:32]
    alog_bc = sm[:, 32:64]
    dskip_bc = sm[:, 64:96]
    sinks_bc = sm[:, 96:112]
    brt_bc = sm[:, 112:184]
    ebase_bc = sm[:, 184:248]
    a_bc = sb("a_bc", [128, 32], F32)
    cw = sb("cw", [128, 96], F32)
    cb = sb("cb", [128, 24], F32)
    diag = sb("diag", [128, 96, 128], BF16)
    Hst = sb("Hst", [128, D], F32)
    carry = sb("carry", [128, 24, 3], BF16)
    PS = es.enter_context(nc.psum_tensor("PS", [128, 4096], F32))

    def bank(b, n=512, nb=1):
        return PS[:, b * 512:b * 512 + n]

    def bank_bf(b, nbanks=2):
        return PS[:, b * 512:(b + nbanks) * 512].bitcast(BF16)

    def PK(b):
        return ("ps", b)

    S.dma("sp", lambda q: q.dma_start(out=cst[:], in_=consts), writes=["cst"])
    S.dma("sp", lambda q: q.dma_start(out=sm[:], in_=small), writes=["sm"])
    S.dma("sp", lambda q: q.dma_start(out=cw[:], in_=convw), writes=["cw"])
    S.dma("sp", lambda q: q.dma_start(out=cb[:], in_=convb), writes=["cb"])
    S.op("dve", lambda: nc.vector.tensor_copy(out=ident_b[:], in_=ident_f), reads=["cst"], writes=["ident_b"])
    S.op("dve", lambda: nc.vector.tensor_copy(out=ones_b[:], in_=ones_f), reads=["cst"], writes=["ones_b"])
    S.op("act", lambda: nc.scalar.activation(out=a_bc[:], in_=alog_bc, func=AF.Exp), reads=["sm"], writes=["a_bc"])
    S.op("dve", lambda: nc.vector.tensor_scalar(out=a_bc[:], in0=a_bc[:], scalar1=-1.0, scalar2=None, op0=ALU.mult),
         reads=["a_bc"], writes=["a_bc"])
    for c in range(24):
        for k in range(4):
            i = c * 4 + k
            S.op("dve", lambda i=i: nc.vector.tensor_scalar(out=diag[:, i, :], in0=ident_f, scalar1=cw[:, i:i + 1],
                                                             scalar2=None, op0=ALU.mult),
                 reads=["cst", "cw"], writes=[("diag", i)])
    S.op("dve", lambda: nc.vector.memset(Hst[:], 0.0), writes=["Hst"])
    S.op("dve", lambda: nc.vector.memset(carry[:], 0.0), writes=["carry"])
    S.barrier()

    class LNres:
        pass

    def make_ln(stack, tag, gcol):
        r = LNres()
        r.g = sb(tag + "_g", [128, D], F32, stack)
        r.b = sb(tag + "_b", [128, D], F32, stack)
        r.st = [sb(tag + "_st%d" % i, [128, 4, 6], F32, stack) for i in range(3)]
        r.mv = [sb(tag + "_mv%d" % i, [128, 2], F32, stack) for i in range(3)]
        r.sd = [sb(tag + "_sd%d" % i, [128, 4], F32, stack) for i in range(3)]
        r.tag = tag
        S.dma("sp", lambda q: q.dma_start(out=r.g[:], in_=lnp[:, gcol * D:(gcol + 1) * D]), writes=[tag + "_g"])
        S.dma("sp", lambda q: q.dma_start(out=r.b[:], in_=lnp[:, (gcol + 1) * D:(gcol + 2) * D]), writes=[tag + "_b"])
        return r

    def layer_norm(r, i, xin, xin_key, yout, yout_key):
        t = r.tag
        st, mv, sd = r.st[i], r.mv[i], r.sd[i]
        for j in range(4):
            S.op("dve", lambda j=j: nc.vector.bn_stats(out=st[:, j, :], in_=xin[:, j * 512:(j + 1) * 512]),
                 reads=[xin_key], writes=[(t, "st", i, j)])
        S.op("dve", lambda: nc.vector.bn_aggr(out=mv[:], in_=st[:].rearrange("p a b -> p (a b)")),
             reads=[(t, "st", i, j) for j in range(4)], writes=[(t, "mv", i)])
        S.op("act", lambda: nc.scalar.activation(out=sd[:, 0:1], in_=mv[:, 1:2], func=AF.Sqrt, bias=LN_EPS, scale=1.0),
             reads=[(t, "mv", i)], writes=[(t, "sd0", i)])
        S.op("dve", lambda: nc.vector.reciprocal(out=sd[:, 1:2], in_=sd[:, 0:1]),
             reads=[(t, "sd0", i)], writes=[(t, "sd1", i)])
        S.op("dve", lambda: nc.vector.tensor_scalar(out=sd[:, 2:3], in0=mv[:, 0:1], scalar1=sd[:, 1:2], scalar2=-1.0,
                                                    op0=ALU.mult, op1=ALU.mult),
             reads=[(t, "mv", i), (t, "sd1", i)], writes=[(t, "sd2", i)])
        S.op("act", lambda: nc.scalar.activation(out=yout, in_=xin, func=AF.Identity, bias=sd[:, 2:3], scale=sd[:, 1:2]),
             reads=[xin_key, (t, "sd1", i), (t, "sd2", i)], writes=[yout_key])
        S.op("dve", lambda: nc.vector.tensor_tensor(out=yout, in0=yout, in1=r.g[:], op=ALU.mult),
             reads=[yout_key, t + "_g"], writes=[yout_key])
        S.op("dve", lambda: nc.vector.tensor_tensor(out=yout, in0=yout, in1=r.b[:], op=ALU.add),
             reads=[yout_key, t + "_b"], writes=[yout_key])

    def ln_to_uT(r, stack_bufs, i, src_rows, uT, tcol, U_rows=None):
        xin, un, ub = stack_bufs["xin"][i], stack_bufs["un"][i], stack_bufs["ub"][i]
        S.dma("sp", lambda q: q.dma_start(out=xin[:], in_=src_rows), writes=[("xin", i)])
        layer_norm(r, i, xin[:], ("xin", i), un[:], ("un", i))
        if U_rows is not None:
            S.dma("sp", lambda q: q.dma_start(out=U_rows, in_=un[:]), reads=[("un", i)], writes=[])
        S.op("act", lambda: nc.scalar.copy(out=ub[:], in_=un[:]), reads=[("un", i)], writes=[("ub", i)])
        pb = (6, 4, 2)[i]
        pv = bank_bf(pb)
        for k in range(KC):
            S.op("pe", lambda k=k: nc.tensor.transpose(out=pv[:, k * 128:(k + 1) * 128], in_=ub[:, k * 128:(k + 1) * 128],
                                                       identity=ident_b[:]),
                 reads=[("ub", i), "ident_b"], writes=[PK(pb), PK(pb + 1)])
        S.op("act", lambda: nc.scalar.copy(out=uT[:, :, tcol:tcol + 128], in_=pv.rearrange("p (k t) -> p k t", k=KC)),
             reads=[PK(pb), PK(pb + 1)], writes=[("uT", tcol // 128)])

    class WRing:
        def __init__(self, stack, tag, shape, n):
            self.t = [sb("%s%d" % (tag, i), shape, BF16, stack) for i in range(n)]
            self.tag = tag
            self.i = 0

        def next(self):
            j = self.i
            self.i = (self.i + 1) % len(self.t)
            return self.t[j], (self.tag, j)

    def load_w_bf(ring, src, kchunks):
        t, key = ring.next()
        n = src.shape[1]
        S.dma("pool", lambda q: q.dma_start(out=t[:, 0:kchunks, 0:n], in_=src.rearrange("(k p) n -> p k n", p=128)),
              writes=[key])
        return t, key

    def tiles_of(nt):
        res = []
        t0 = 0
        while t0 < nt:
            n = min(512, nt - t0)
            res.append((t0, n))
            t0 += n
        return res

    psrot = {"i": 0}

    def next_bank(lo=1, hi=4):
        b = lo + psrot["i"] % (hi - lo)
        psrot["i"] += 1
        return b

    def proj_fm(wt, wkey, uT, ukeys, t0, n, b, kchunks=KC):
        for k in range(kchunks):
            S.op("pe", lambda k=k: nc.tensor.matmul(bank(b, n), lhsT=wt[:, k, 0:128], rhs=uT[:, k, t0:t0 + n],
                                                    start=(k == 0), stop=(k == kchunks - 1)),
                 reads=[wkey] + ukeys, writes=[PK(b)])

    def conv_chunk(cc, wfm_ring, uT, ukeys_fn, nt, tok_off, xpre, xkey, xcT, xckey, mask_tile, mask_key, tiles=None):
        wt, wkey = load_w_bf(wfm_ring, w_in[:, OFF_X + cc * 128:OFF_X + (cc + 1) * 128], KC)
        S.op("act", lambda: nc.scalar.copy(out=xpre[:, 0:3], in_=carry[:, cc, :]), reads=[("carry", cc)], writes=[xkey])
        tl = tiles or tiles_of(nt)
        for (t0, n) in tl:
            b = next_bank()
            proj_fm(wt, wkey, uT, ukeys_fn(t0, n), tok_off + t0, n, b)
            if mask_tile is not None and t0 == 0:
                S.op("dve", lambda t0=t0, n=n, b=b: nc.vector.tensor_tensor(
                    out=xpre[:, 3 + t0:3 + t0 + n], in0=bank(b, n), in1=mask_tile[:, t0:t0 + n], op=ALU.mult),
                    reads=[PK(b), mask_key], writes=[xkey])
            else:
                S.op("dve", lambda t0=t0, n=n, b=b: nc.vector.tensor_copy(out=xpre[:, 3 + t0:3 + t0 + n], in_=bank(b, n)),
                     reads=[PK(b)], writes=[xkey])
        S.op("act", lambda: nc.scalar.copy(out=carry[:, cc, :], in_=xpre[:, nt:nt + 3]), reads=[xkey], writes=[("carry", cc)])
        for (t0, n) in tl:
            b = next_bank()
            for k in range(4):
                S.op("pe", lambda k=k, t0=t0, n=n, b=b: nc.tensor.matmul(
                    bank(b, n), lhsT=diag[:, cc * 4 + k, :], rhs=xpre[:, t0 + k:t0 + k + n], start=(k == 0), stop=(k == 3)),
                    reads=[xkey, ("diag", cc * 4 + k)], writes=[PK(b)])
            S.op("act", lambda t0=t0, n=n, b=b: nc.scalar.activation(out=xcT[:, t0:t0 + n], in_=bank(b, n), func=AF.Silu,
                                                                   bias=cb[:, cc:cc + 1], scale=1.0),
                 reads=[PK(b), "cb"], writes=[xckey])

    def dt_chunk(wdt, uT, ukeys, tcol, dtout, dtkey, mask_col=None, mask_key=None):
        b = next_bank()
        for k in range(KC):
            S.op("pe", lambda k=k: nc.tensor.matmul(bank(b, 32), lhsT=uT[:, k, tcol:tcol + 128], rhs=wdt[:, k, 0:32],
                                                    start=(k == 0), stop=(k == KC - 1)),
                 reads=["wdt"] + ukeys, writes=[PK(b)])
        S.op("dve", lambda: nc.vector.tensor_tensor(out=dtout, in0=bank(b, 32), in1=dtb_bc, op=ALU.add),
             reads=[PK(b), "sm"], writes=[dtkey])
        S.op("act", lambda: nc.scalar.activation(out=dtout, in_=dtout, func=AF.Exp), reads=[dtkey], writes=[dtkey])
        S.op("act", lambda: nc.scalar.activation(out=dtout, in_=dtout, func=AF.Ln, bias=1.0, scale=1.0),
             reads=[dtkey], writes=[dtkey])
        if mask_col is not None:
            S.op("dve", lambda: nc.vector.tensor_scalar(out=dtout, in0=dtout, scalar1=mask_col, scalar2=None, op0=ALU.mult),
                 reads=[dtkey, mask_key], writes=[dtkey])

    def ssd_state_update(tmp, xs_c, xs_keys, dt_c, dt_key, totsum=None):
        adt, dte, dA, scl, xdtd = tmp["adt"], tmp["dte"], tmp["dA"], tmp["scl"], tmp["xdtd"]
        S.op("dve", lambda: nc.vector.tensor_tensor(out=adt[:], in0=dt_c, in1=a_bc[:], op=ALU.mult),
             reads=[dt_key, "a_bc"], writes=["adt"])
        S.op("pe", lambda: nc.tensor.matmul(PS[:, 0:32], lhsT=triU, rhs=adt[:], start=True, stop=True),
             reads=["adt", "cst"], writes=[PK(0)])
        S.op("pe", lambda: nc.tensor.matmul(PS[:, 32:64], lhsT=ones_f, rhs=adt[:], start=True, stop=True),
             reads=["adt", "cst"], writes=[PK(0)])
        S.op("act", lambda: nc.scalar.activation(out=dte[:], in_=PS[:, 0:32], func=AF.Exp),
             reads=[PK(0)], writes=["dte"])
        S.op("act", lambda: nc.scalar.activation(out=dA[:], in_=PS[:, 32:64], func=AF.Exp),
             reads=[PK(0)], writes=["dA"])
        if totsum is not None:
            S.op("dve", lambda: nc.vector.tensor_tensor(out=totsum[:], in0=totsum[:], in1=PS[:, 32:64], op=ALU.add),
                 reads=[PK(0), "totsum"], writes=["totsum"])
        S.op("dve", lambda: nc.vector.tensor_tensor(out=scl[:], in0=dt_c, in1=dte[:], op=ALU.mult),
             reads=[dt_key, "dte"], writes=["scl"])
        S.op("dve", lambda: nc.vector.tensor_tensor(
            out=xdtd[:].rearrange("p (h d) -> p h d", h=32), in0=xs_c[:, 0:2048].rearrange("p (h d) -> p h d", h=32),
            in1=scl[:].rearrange("p (h o) -> p h o", o=1).to_broadcast([128, 32, 64]), op=ALU.mult),
            reads=list(xs_keys) + ["scl"], writes=["xdtd"])
        for g in range(4):
            S.op("pe", lambda g=g: nc.tensor.matmul(bank(4 + g), lhsT=xs_c[:, 2048 + g * 128:2048 + (g + 1) * 128],
                                                    rhs=xdtd[:, g * 512:(g + 1) * 512], start=True, stop=True),
                 reads=list(xs_keys) + ["xdtd"], writes=[PK(4 + g)])
        S.op("dve", lambda: nc.vector.tensor_tensor(
            out=Hst[:].rearrange("p (h d) -> p h d", h=32), in0=Hst[:].rearrange("p (h d) -> p h d", h=32),
            in1=dA[:].rearrange("p (h o) -> p h o", o=1).to_broadcast([128, 32, 64]), op=ALU.mult),
            reads=["Hst", "dA"], writes=["Hst"])
        S.op("dve", lambda: nc.vector.tensor_tensor(out=Hst[:], in0=Hst[:], in1=PS[:, 2048:4096], op=ALU.add),
             reads=["Hst"] + [PK(4 + g) for g in range(4)], writes=["Hst"])

    rtw = sb("rtw", [128, 16, 2], F32)
    rts = sb("rts", [128, 16, 2], I32)
    cnt_bc = sb("cnt_bc", [128, 64], F32)

    with ExitStack() as ph:
        NTB = NB_CH * 128
        uTB = sb("uTB", [128, KC, NTB], BF16, ph)
        with ExitStack() as p1:
            lnr = make_ln(p1, "lnB", 0)
            lb = {"xin": [sb("xinB%d" % i, [128, D], F32, p1) for i in range(3)],
                  "un": [sb("unB%d" % i, [128, D], F32, p1) for i in range(3)],
                  "ub": [sb("ubB%d" % i, [128, D], BF16, p1) for i in range(3)]}
            for c in range(NB_CH):
                ln_to_uT(lnr, lb, c % 3, xb[c * 128:(c + 1) * 128, :], uTB, c * 128,
                         U[(c - 2) * 128:(c - 1) * 128, :] if c >= 2 else None)
            S.barrier()
        allu = [("uT", c) for c in range(NB_CH)]
        wfm = WRing(ph, "wfmB", [128, KC, 128], 3)
        wtm = WRing(ph, "wtmB", [128, KC, 512], 1)
        wdt = sb("wdtB", [128, KC, 32], BF16, ph)
        xpre = [sb("xpreB%d" % i, [128, 17 * 128 + 3], BF16, ph) for i in range(2)]
        xcT = [sb("xcTB%d" % i, [128, 17 * 128], BF16, ph) for i in range(2)]
        stg = [sb("stgB%d" % i, [128, NTB], BF16, ph) for i in range(2)]
        stgT = [sb("stgTB0", [128, 17, 128], BF16, ph)] * 2
        zstg = sb("zstg", [128, 16, 512], BF16, ph)
        vstg = sb("vstg", [128, NB_CH, 256], BF16, ph)
        dtstg = sb("dtstg", [128, 17, 32], F32, ph)
        S.dma("pool", lambda q: q.dma_start(out=wdt[:], in_=w_in[:, OFF_DT:OFF_DT + 32].rearrange("(k p) n -> p k n", p=128)),
              writes=["wdt"])
        si = {"i": 0}

        def fm_out(col0, dst_rows, tok_off, nt, func):
            i = si["i"] % 2
            si["i"] += 1
            wt, wkey = load_w_bf(wfm, w_in[:, col0:col0 + 128], KC)
            for (t0, n) in tiles_of(nt):
                b = next_bank()
                proj_fm(wt, wkey, uTB, allu, tok_off + t0, n, b)
                S.op("act", lambda t0=t0, n=n, b=b, i=i: nc.scalar.activation(out=stg[i][:, t0:t0 + n], in_=bank(b, n), func=func),
                     reads=[PK(b)], writes=[("stg", i)])
            S.dma("sp", lambda q, i=i: q.dma_start(out=dst_rows, in_=stg[i][:, 0:nt]), reads=[("stg", i)], writes=[])

        for cc in range(8):
            fm_out(OFF_Q + cc * 128, Q_T[cc * 128:(cc + 1) * 128, :], 256, OWN, AF.Copy)
        for cc in range(2):
            fm_out(OFF_K + cc * 128, K_T[cc * 128:(cc + 1) * 128, :], 0, NTB, AF.Copy)
        for cc in range(32):
            fm_out(OFF_GA + cc * 128, G_T[cc * 128:(cc + 1) * 128, :], 256, OWN, AF.Sigmoid)
        hm = sb("hm", [128, 137], F32, ph)
        S.dma("sp", lambda q: q.dma_start(out=hm[:], in_=hmask), writes=["hm"])
        NTX = 17 * 128
        tilesB = [(0, 128)] + [(128 + 512 * k, 512) for k in range(4)]
        for cc in range(24):
            i = cc % 2
            conv_chunk(cc, wfm, uTB, lambda t0, n: allu, NTX, 128, xpre[i], ("xpre", i), xcT[i], ("xcT", i), hm[:, 0:128], "hm",
                       tiles=tilesB)
            if cc >= 16:
                S.dma("sp", lambda q, i=i, cc=cc: q.dma_start(out=BC_T[(cc - 16) * 128:(cc - 15) * 128, :], in_=xcT[i][:, 128:NTX]),
                      reads=[("xcT", i)], writes=[])
            if cc < 20:
                for (pb, c0, c1) in ((6, 0, 9), (4, 9, 17)):
                    pv = bank_bf(pb)
                    for c in range(c0, c1):
                        S.op("pe", lambda c=c, i=i, pv=pv, c0=c0: nc.tensor.transpose(
                            out=pv[:, (c - c0) * 128:(c - c0 + 1) * 128], in_=xcT[i][:, c * 128:(c + 1) * 128], identity=ident_b[:]),
                            reads=[("xcT", i), "ident_b"], writes=[PK(pb), PK(pb + 1)])
                    S.op("act", lambda pv=pv, c0=c0, c1=c1: nc.scalar.copy(
                        out=stgT[0][:, c0:c1, :], in_=pv[:, 0:(c1 - c0) * 128].rearrange("p (c t) -> p c t", c=c1 - c0)),
                        reads=[PK(pb), PK(pb + 1)], writes=[("stgT", c0)])
                S.dma("sp", lambda q, cc=cc: q.dma_start(
                    out=XS[:, cc * 128:(cc + 1) * 128].rearrange("(c p) n -> p c n", p=128), in_=stgT[0][:]),
                    reads=[("stgT", 0), ("stgT", 9)], writes=[])
        for c in range(17):
            if c == 0:
                dt_chunk(wdt, uTB, allu, (c + 1) * 128, dtstg[:, c, :], ("dtstg", c), hm[:, 128:129], "hm")
            else:
                dt_chunk(wdt, uTB, allu, (c + 1) * 128, dtstg[:, c, :], ("dtstg", c))
        S.dma("sp", lambda q: q.dma_start(out=DT_.rearrange("(c p) h -> p c h", p=128), in_=dtstg[:]),
              reads=[("dtstg", c) for c in range(17)], writes=[])
        wv, wvkey = load_w_bf(wtm, w_in[:, OFF_V:OFF_V + 256], KC)
        for c in range(NB_CH):
            b = next_bank()
            for k in range(KC):
                S.op("pe", lambda k=k, c=c, b=b: nc.tensor.matmul(bank(b, 256), lhsT=uTB[:, k, c * 128:(c + 1) * 128],
                                                                 rhs=wv[:, k, 0:256], start=(k == 0), stop=(k == KC - 1)),
                     reads=[wvkey] + allu, writes=[PK(b)])
            S.op("act", lambda c=c, b=b: nc.scalar.copy(out=vstg[:, c, :], in_=bank(b, 256)), reads=[PK(b)], writes=[("vstg", c)])
        S.dma("sp", lambda q: q.dma_start(out=V_.rearrange("(c p) n -> p c n", p=128), in_=vstg[:]),
              reads=[("vstg", c) for c in range(NB_CH)], writes=[])
        for un in range(4):
            wz, wzkey = load_w_bf(wtm, w_in[:, OFF_Z + un * 512:OFF_Z + (un + 1) * 512], KC)
            for c in range(16):
                b = next_bank()
                for k in range(KC):
                    S.op("pe", lambda k=k, c=c, b=b, wz=wz: nc.tensor.matmul(
                        bank(b), lhsT=uTB[:, k, (c + 2) * 128:(c + 3) * 128], rhs=wz[:, k, :], start=(k == 0), stop=(k == KC - 1)),
                        reads=[wzkey] + allu, writes=[PK(b)])
                S.op("act", lambda c=c, b=b: nc.scalar.copy(out=zstg[:, c, :], in_=bank(b)), reads=[PK(b)], writes=[("zstg", c)])
            S.dma("sp", lambda q, un=un: q.dma_start(
                out=Z_[:, un * 512:(un + 1) * 512].rearrange("(c p) n -> p c n", p=128), in_=zstg[:]),
                reads=[("zstg", c) for c in range(16)], writes=[])
        S.barrier()
    if stop_after == "B2":
        es.close()
        return nc

    with ExitStack() as ph:
        xs_r = [sb("xsA%d" % i, [128, 2560], BF16, ph) for i in range(2)]
        dtc_r = [sb("dtcA%d" % i, [128, 32], F32, ph) for i in range(2)]
        tmp = {"adt": sb("adtA", [128, 32], F32, ph), "dte": sb("dteA", [128, 32], F32, ph),
               "dA": sb("dAA", [128, 32], F32, ph), "scl": sb("sclA", [128, 32], F32, ph),
               "xdtd": sb("xdtdA", [128, 2048], BF16, ph)}
        totsum = sb("totsum", [128, 32], F32, ph)
        hmA = sb("hmA", [128, 137], F32, ph)
        gr = [sb("gr%d" % i, [128, 2080], F32, ph) for i in range(2)]
        dsel = sb("dsel", [128, 32], F32, ph)
        S.dma("sp", lambda q: q.dma_start(out=hmA[:], in_=hmask), writes=["hmA"])
        S.op("dve", lambda: nc.vector.memset(totsum[:], 0.0), writes=["totsum"])
        for c in range(17):
            i = c % 2
            S.dma("sp", lambda q: q.dma_start(out=xs_r[i][:], in_=XS[c * 128:(c + 1) * 128, :]), writes=[("xsA", i)])
            S.dma("sp", lambda q: q.dma_start(out=dtc_r[i][:], in_=DT_[c * 128:(c + 1) * 128, :]), writes=[("dtcA", i)])
            ssd_state_update(tmp, xs_r[i][:], [("xsA", i)], dtc_r[i][:], ("dtcA", i), totsum)
        S.op("act", lambda: nc.scalar.activation(out=totsum[:], in_=totsum[:], func=AF.Exp), reads=["totsum"], writes=["totsum"])
        S.dma("sp", lambda q: q.dma_start(out=GSRC[:, 0:2048], in_=Hst[:]), reads=["Hst"], writes=["GSRC"])
        S.dma("sp", lambda q: q.dma_start(out=GSRC[:, 2048:2080], in_=totsum[:]), reads=["totsum"], writes=["GSRC"])
        if nocc:
            S.dma("sp", lambda q: q.dma_start(out=GDST[0:128, :], in_=GSRC), reads=["GSRC"], writes=["GDST"])
        else:
            S.collective(lambda: nc.gpsimd.collective_compute("AllGather", ALU.bypass, replica_groups=[list(range(8))],
                                                              ins=[GSRC], outs=[GDST]),
                         reads=["GSRC"], writes=["GDST"])
        S.op("dve", lambda: nc.vector.memset(Hst[:], 0.0), reads=["Hst"], writes=["Hst"])
        for r8 in range(8):
            i = r8 % 2
            S.dma("sp", lambda q: q.dma_start(out=gr[i][:], in_=GDST[r8 * 128:(r8 + 1) * 128, :]), reads=["GDST"], writes=[("gr", i)])
            selr = hmA[:, 129 + r8:130 + r8]
            S.op("dve", lambda: nc.vector.tensor_scalar(out=dsel[:], in0=gr[i][:, 2048:2080], scalar1=-1.0, scalar2=selr,
                                                        op0=ALU.add, op1=ALU.mult),
                 reads=[("gr", i), "hmA"], writes=["dsel"])
            S.op("dve", lambda: nc.vector.tensor_scalar(out=dsel[:], in0=dsel[:], scalar1=1.0, scalar2=None, op0=ALU.add),
                 reads=["dsel"], writes=["dsel"])
            S.op("dve", lambda: nc.vector.tensor_tensor(
                out=Hst[:].rearrange("p (h d) -> p h d", h=32), in0=Hst[:].rearrange("p (h d) -> p h d", h=32),
                in1=dsel[:].rearrange("p (h o) -> p h o", o=1).to_broadcast([128, 32, 64]), op=ALU.mult),
                reads=["Hst", "dsel"], writes=["Hst"])
            S.op("dve", lambda: nc.vector.scalar_tensor_tensor(out=Hst[:], in0=gr[i][:, 0:2048], scalar=selr, in1=Hst[:],
                                                               op0=ALU.mult, op1=ALU.add),
                 reads=["Hst", ("gr", i), "hmA"], writes=["Hst"])
        S.dma("sp", lambda q: q.dma_start(out=xs_r[0][:], in_=XS[0:128, :]), writes=[("xsA", 0)])
        S.dma("sp", lambda q: q.dma_start(out=dtc_r[0][:], in_=DT_[0:128, :]), writes=[("dtcA", 0)])
        ssd_state_update(tmp, xs_r[0][:], [("xsA", 0)], dtc_r[0][:], ("dtcA", 0))
        if "HDBG" in dbg:
            S.dma("sp", lambda q: q.dma_start(out=HDBG, in_=Hst[:]), reads=["Hst"], writes=["HDBG"])
        S.barrier()
    if stop_after == "A":
        es.close()
        return nc

    with ExitStack() as ph:
        gn = sb("gn", [128, D], F32, ph)
        S.dma("sp", lambda q: q.dma_start(out=gn[:], in_=gnorm), writes=["gn"])
        xs_r = [sb("xs_r%d" % i, [128, 2560], BF16, ph) for i in range(2)]
        bct_r = [sb("bct_r%d" % i, [128, 8, 128], BF16, ph) for i in range(2)]
        dtc_r = [sb("dtc_r%d" % i, [128, 32], F32, ph) for i in range(2)]
        zc_r = [sb("zc_r%d" % i, [128, D], BF16, ph) for i in range(2)]
        tmp = {"adt": sb("adt3", [128, 32], F32, ph), "dte": sb("dte3", [128, 32], F32, ph),
               "dA": sb("dA3", [128, 32], F32, ph), "scl": sb("scl3", [128, 32], F32, ph),
               "xdtd": sb("xdtd3", [128, 2048], BF16, ph)}
        adtb = sb("adtb", [128, 32], F32, ph)
        nacs = sb("nacs", [128, 32], F32, ph)
        eacs = sb("eacs", [128, 32], F32, ph)
        xdt = sb("xdt", [128, D], BF16, ph)
        Hbf = sb("Hbf", [128, D], BF16, ph)
        Eh = [sb("Eh%d" % i, [128, 128], F32, ph) for i in range(8)]
        Gh = [sb("Gh%d" % i, [128, 128], BF16, ph) for i in range(8)]
        yo = sb("yo", [128, D], F32, ph)
        ysb = sb("ysb", [128, D], F32, ph)
        sz = sb("sz", [128, D], F32, ph)
        sqs = sb("sqs", [128, 512], F32, ph)
        ss = sb("ss", [128, 8], F32, ph)
        yn = sb("yn", [128, D], BF16, ph)
        ynT = [sb("ynT%d" % i, [128, 16, 128], BF16, ph) for i in range(2)]
        h3 = lambda ap: ap.rearrange("p (h d) -> p h d", h=32)
        b3 = lambda ap: ap.rearrange("p (h o) -> p h o", o=1).to_broadcast([128, 32, 64])
        PE_KEYS = [PK(2), PK(3)]
        for c in range(16):
            i = c % 2
            xs_c, bct, dtc, zc = xs_r[i], bct_r[i], dtc_r[i], zc_r[i]
            S.dma("sp", lambda q: q.dma_start(out=xs_c[:], in_=XS[(c + 1) * 128:(c + 2) * 128, :]), writes=[("xs_r", i)])
            S.dma("sp", lambda q: q.dma_start(out=bct[:], in_=BC_T[:, c * 128:(c + 1) * 128].rearrange("(g p) t -> p g t", p=128)),
                  writes=[("bct", i)])
            S.dma("sp", lambda q: q.dma_start(out=dtc[:], in_=DT_[(c + 1) * 128:(c + 2) * 128, :]), writes=[("dtc", i)])
            S.dma("sp", lambda q: q.dma_start(out=zc[:], in_=Z_[c * 128:(c + 1) * 128, :]), writes=[("zc", i)])
            S.op("dve", lambda: nc.vector.tensor_tensor(out=adtb[:], in0=dtc[:], in1=a_bc[:], op=ALU.mult),
                 reads=[("dtc", i), "a_bc"], writes=["adtb"])
            S.op("pe", lambda: nc.tensor.matmul(PS[:, 64:96], lhsT=tri_le, rhs=adtb[:], start=True, stop=True),
                 reads=["adtb", "cst"], writes=[PK(0)])
            S.op("act", lambda: nc.scalar.activation(out=nacs[:], in_=PS[:, 64:96], func=AF.Copy, scale=-1.0),
                 reads=[PK(0)], writes=["nacs"])
            S.op("act", lambda: nc.scalar.activation(out=eacs[:], in_=PS[:, 64:96], func=AF.Exp),
                 reads=[PK(0)], writes=["eacs"])
            S.op("dve", lambda: nc.vector.tensor_tensor(out=h3(xdt[:]), in0=h3(xs_c[:, 0:2048]), in1=b3(dtc[:]), op=ALU.mult),
                 reads=[("xs_r", i), ("dtc", i)], writes=["xdt"])
            S.op("act", lambda: nc.scalar.copy(out=Hbf[:], in_=Hst[:]), reads=["Hst"], writes=["Hbf"])
            for g in range(4):
                S.op("pe", lambda g=g: nc.tensor.matmul(bank(4 + g), lhsT=bct[:, 4 + g, :], rhs=Hbf[:, g * 512:(g + 1) * 512],
                                                        start=True, stop=True),
                     reads=[("bct", i), "Hbf"], writes=[PK(4 + g)])
            S.op("dve", lambda: nc.vector.tensor_tensor(out=h3(yo[:]), in0=h3(PS[:, 2048:4096]), in1=b3(eacs[:]), op=ALU.mult),
                 reads=[PK(4 + g) for g in range(4)] + ["eacs"], writes=["yo"])
            for g in range(4):
                S.op("pe", lambda g=g: nc.tensor.matmul(PS[:, 512 + g * 128:512 + (g + 1) * 128], lhsT=bct[:, g, :], rhs=bct[:, 4 + g, :],
                                                        start=True, stop=True),
                     reads=[("bct", i)], writes=[PK(1)])
            for bt in range(9):
                if bt < 8:
                    bk = 2 + bt % 2
                    for q4 in range(4):
                        h = bt * 4 + q4
                        pe_out = PS[:, bk * 512 + q4 * 128:bk * 512 + (q4 + 1) * 128]
                        S.op("pe", lambda h=h, pe_out=pe_out: nc.tensor.matmul(
                            pe_out, lhsT=adtb[:, h:h + 1].to_broadcast([128, 128]), rhs=tri_le, start=True, stop=False),
                            reads=["adtb", "cst"], writes=[PK(bk)])
                        S.op("pe", lambda pe_out=pe_out: nc.tensor.matmul(pe_out, lhsT=ident_f, rhs=maskneg, start=False, stop=True),
                             reads=["cst"], writes=[PK(bk)])
                    for q4 in range(4):
                        h = bt * 4 + q4
                        g = h // 8
                        r4 = h % 8
                        pe_out = PS[:, bk * 512 + q4 * 128:bk * 512 + (q4 + 1) * 128]
                        S.op("act", lambda h=h, r4=r4, pe_out=pe_out: nc.scalar.activation(
                            out=Eh[r4][:], in_=pe_out, func=AF.Exp, bias=nacs[:, h:h + 1], scale=1.0),
                            reads=[PK(bk), "nacs"], writes=[("Eh", r4)])
                        S.op("dve", lambda g=g, r4=r4: nc.vector.tensor_tensor(
                            out=Gh[r4][:], in0=Eh[r4][:], in1=PS[:, 512 + g * 128:512 + (g + 1) * 128], op=ALU.mult),
                            reads=[("Eh", r4), PK(1)], writes=[("Gh", r4)])
                if bt >= 1:
                    for q4 in range(4):
                        h = (bt - 1) * 4 + q4
                        r4 = h % 8
                        S.op("pe", lambda h=h, r4=r4: nc.tensor.matmul(PS[:, 2048 + h * 64:2048 + (h + 1) * 64], lhsT=Gh[r4][:],
                                                                       rhs=xdt[:, h * 64:(h + 1) * 64], start=True, stop=True),
                             reads=[("Gh", r4), "xdt", "yo"], writes=[PK(4 + h // 8)])
            S.op("dve", lambda: nc.vector.tensor_tensor(out=ysb[:], in0=PS[:, 2048:4096], in1=yo[:], op=ALU.add),
                 reads=[PK(4 + g) for g in range(4)] + ["yo"], writes=["ysb"])
            S.op("dve", lambda: nc.vector.tensor_tensor(out=h3(yo[:]), in0=h3(xs_c[:, 0:2048]), in1=b3(dskip_bc), op=ALU.mult),
                 reads=[("xs_r", i), "sm", "ysb"], writes=["yo"])
            S.op("dve", lambda: nc.vector.tensor_tensor(out=ysb[:], in0=ysb[:], in1=yo[:], op=ALU.add),
                 reads=["ysb", "yo"], writes=["ysb"])
            S.op("act", lambda: nc.scalar.activation(out=sz[:], in_=zc[:], func=AF.Silu), reads=[("zc", i)], writes=["sz"])
            S.op("dve", lambda: nc.vector.tensor_tensor(out=ysb[:], in0=ysb[:], in1=sz[:], op=ALU.mult),
                 reads=["ysb", "sz"], writes=["ysb"])
            for g in range(4):
                S.op("act", lambda g=g: nc.scalar.activation(out=sqs[:], in_=ysb[:, g * 512:(g + 1) * 512], func=AF.Square,
                                                             accum_out=ss[:, g:g + 1]),
                     reads=["ysb"], writes=["sqs", ("ss", g)])
            S.op("act", lambda: nc.scalar.activation(out=ss[:, 4:8], in_=ss[:, 0:4], func=AF.Sqrt, bias=RMS_EPS, scale=1.0 / 512),
                 reads=[("ss", g) for g in range(4)], writes=["ssd"])
            S.op("dve", lambda: nc.vector.reciprocal(out=ss[:, 4:8], in_=ss[:, 4:8]), reads=["ssd"], writes=["ssd"])
            for g in range(4):
                S.op("dve", lambda g=g: nc.vector.scalar_tensor_tensor(
                    out=yn[:, g * 512:(g + 1) * 512], in0=ysb[:, g * 512:(g + 1) * 512], scalar=ss[:, 4 + g:5 + g],
                    in1=gn[:, g * 512:(g + 1) * 512], op0=ALU.mult, op1=ALU.mult),
                    reads=["ysb", "ssd", "gn"], writes=[("yn", g)])
            pv = bank_bf(2)
            for k in range(KC):
                S.op("pe", lambda k=k, pv=pv: nc.tensor.transpose(out=pv[:, k * 128:(k + 1) * 128], in_=yn[:, k * 128:(k + 1) * 128],
                                                                  identity=ident_b[:]),
                     reads=[("yn", k // 4), "ident_b"], writes=PE_KEYS)
            S.op("act", lambda pv=pv: nc.scalar.copy(out=ynT[i][:], in_=pv.rearrange("p (k t) -> p k t", k=KC)),
                 reads=PE_KEYS, writes=[("ynT", i)])
            S.dma("sp", lambda q: q.dma_start(out=YN_T[:, c * 128:(c + 1) * 128].rearrange("(k p) t -> p k t", p=128), in_=ynT[i][:]),
                  reads=[("ynT", i)], writes=[])
            ssd_state_update(tmp, xs_c[:], [("xs_r", i)], dtc[:], ("dtc", i))
        S.barrier()
    if stop_after == "B3":
        es.close()
        return nc

    with ExitStack() as ph:
        NTB = NB_CH * 128
        mfs = sb("mfs", [128, 256], F32, ph)
        mk = sb("mk", [128, 4, 128], BF16, ph)
        S.dma("sp", lambda q: q.dma_start(out=mfs[:], in_=mfirst), writes=["mfs"])
        S.op("dve", lambda: nc.vector.tensor_copy(out=mk[:, 0, :], in_=mprev_f), reads=["cst"], writes=[("mk", 0)])
        S.op("dve", lambda: nc.vector.tensor_copy(out=mk[:, 1, :], in_=mcur_f), reads=["cst"], writes=[("mk", 1)])
        S.op("dve", lambda: nc.vector.tensor_copy(out=mk[:, 2:4, :], in_=mfs[:].rearrange("p (a t) -> p a t", a=2)),
             reads=["mfs"], writes=[("mk", 2), ("mk", 3)])
        mneg = sb("mneg", [128, 4, 512], BF16, ph)
        for mi in range(4):
            S.op("dve", lambda mi=mi: nc.vector.tensor_scalar(
                out=mneg[:, mi, :].rearrange("p (h t) -> p h t", h=4), in0=mk[:, mi:mi + 1, :].to_broadcast([128, 4, 128]),
                scalar1=-1.0, scalar2=30000.0, op0=ALU.add, op1=ALU.mult),
                reads=[("mk", mi)], writes=[("mneg", mi)])
        ktg = [sb("ktg%d" % i, [64, NTB], BF16, ph) for i in range(2)]
        vtg = [sb("vtg%d" % i, [128, NB_CH, 64], BF16, ph) for i in range(2)]
        qtg = [sb("qtg%d" % i, [64, 4, OWN], BF16, ph) for i in range(2)]
        ostg = [sb("ostg%d" % i, [64, 4, OWN], BF16, ph) for i in range(2)]
        sk = sb("sk", [64, 16], F32, ph)
        Eb = [sb("Eb%d" % i, [128, 512], BF16, ph) for i in range(4)]
        Em = [sb("Em%d" % i, [128, 512], BF16, ph) for i in range(4)]
        dsb = [sb("dsb%d" % i, [64, 512], F32, ph) for i in range(2)]
        S.op("act", lambda: nc.scalar.activation(out=sk[:], in_=sinks_bc[0:64, :], func=AF.Exp), reads=["sm"], writes=["sk"])
        for g in range(4):
            gi = g % 2
            S.dma("sp", lambda q: q.dma_start(out=ktg[gi][:], in_=K_T[g * 64:(g + 1) * 64, :]), writes=[("ktg", gi)])
            S.dma("sp", lambda q: q.dma_start(out=vtg[gi][:], in_=V_[:, g * 64:(g + 1) * 64].rearrange("(c p) d -> p c d", p=128)),
                  writes=[("vtg", gi)])
            S.dma("sp", lambda q: q.dma_start(out=qtg[gi][:], in_=Q_T[g * 256:(g + 1) * 256, :].rearrange("(h d) t -> d h t", d=64)),
                  writes=[("qtg", gi)])
            seq = []
            for blk in range(16):
                for pi, (ch, mi) in enumerate([(1 + blk, 2 if blk == 0 else 0), (2 + blk, 1), (0, 3)]):
                    seq.append((blk, pi, ch, mi))
            LAG = 3
            for idx in range(len(seq) + LAG):
                if idx < len(seq):
                    blk, pi, ch, mi = seq[idx]
                    e3 = idx % 4
                    b = idx % 4
                    S.op("pe", lambda ch=ch, b=b, blk=blk: nc.tensor.matmul(bank(b), lhsT=ktg[gi][:, ch * 128:(ch + 1) * 128],
                                                                  rhs=qtg[gi][:, :, blk * 128:(blk + 1) * 128], start=True, stop=False),
                         reads=[("ktg", gi), ("qtg", gi)], writes=[PK(b)])
                    S.op("pe", lambda b=b, mi=mi: nc.tensor.matmul(bank(b), lhsT=ident_b[:], rhs=mneg[:, mi, :], start=False, stop=True),
                         reads=["ident_b", ("mneg", mi)], writes=[PK(b)])
                    S.op("act", lambda b=b, e3=e3: nc.scalar.activation(out=Em[e3][:], in_=bank(b), func=AF.Exp, scale=0.125),
                         reads=[PK(b)], writes=[("Em", e3)])
                if idx >= LAG:
                    blk, pi, ch, mi = seq[idx - LAG]
                    e3 = (idx - LAG) % 4
                    bi = blk % 2
                    nb, db = 4 + 2 * bi, 5 + 2 * bi
                    S.op("pe", lambda ch=ch, e3=e3, pi=pi, nb=nb: nc.tensor.matmul(PS[0:64, nb * 512:(nb + 1) * 512], lhsT=vtg[gi][:, ch, :],
                                                                         rhs=Em[e3][:], start=(pi == 0), stop=(pi == 2)),
                         reads=[("vtg", gi), ("Em", e3)], writes=[PK(nb)])
                    S.op("pe", lambda e3=e3, pi=pi, db=db: nc.tensor.matmul(PS[0:64, db * 512:(db + 1) * 512], lhsT=ones_b[:, 0:64],
                                                                   rhs=Em[e3][:], start=(pi == 0), stop=(pi == 2)),
                         reads=["ones_b", ("Em", e3)], writes=[PK(db)])
                    if pi == 2:
                        S.op("dve", lambda bi=bi, db=db: nc.vector.tensor_tensor(
                            out=dsb[bi][:].rearrange("p (h t) -> p h t", h=4), in0=PS[0:64, db * 512:(db + 1) * 512].rearrange("p (h t) -> p h t", h=4),
                            in1=sk[:, g * 4:(g + 1) * 4].rearrange("p (h o) -> p h o", o=1).to_broadcast([64, 4, 128]), op=ALU.add),
                            reads=[PK(db), "sk"], writes=[("dsb", bi)])
                        S.op("dve", lambda bi=bi: nc.vector.reciprocal(out=dsb[bi][:], in_=dsb[bi][:]), reads=[("dsb", bi)], writes=[("dsb", bi)])
                        S.op("dve", lambda bi=bi, nb=nb, blk=blk: nc.vector.tensor_tensor(
                            out=ostg[gi][:, :, blk * 128:(blk + 1) * 128], in0=PS[0:64, nb * 512:(nb + 1) * 512].rearrange("p (h t) -> p h t", h=4),
                            in1=dsb[bi][:].rearrange("p (h t) -> p h t", h=4), op=ALU.mult),
                            reads=[PK(nb), ("dsb", bi)], writes=[("ostg", gi)])
            S.dma("sp", lambda q: q.dma_start(out=AT_T[g * 256:(g + 1) * 256, :].rearrange("(h d) t -> d h t", d=64), in_=ostg[gi][:]),
                  reads=[("ostg", gi)], writes=[])
        S.barrier()
    if stop_after == "B4":
        es.close()
        return nc

    with ExitStack() as ph:
        at = [sb("at%d" % i, [128, 8, 512], BF16, ph) for i in range(2)]
        yt = [sb("yt0", [128, 16, 512], BF16, ph)] * 2
        wr5 = WRing(ph, "w5", [128, KC, 512], 4)
        gag = [sb("gag%d" % i, [128, 4, 512], BF16, ph) for i in range(2)]
        gsg = [sb("gsg%d" % i, [128, 4, 512], BF16, ph) for i in range(2)]
        m1 = [sb("m1_%d" % i, [128, 512], F32, ph) for i in range(2)]
        m2 = [sb("m2_%d" % i, [128, 512], F32, ph) for i in range(2)]
        mst = [sb("mst%d" % i, [128, 16, 512], BF16, ph) for i in range(2)]
        zt = sb("zt", [128, D], BF16, ph)
        S.op("dve", lambda: nc.vector.memset(zt[:], 0.0), writes=["zt"])
        for e in range(64):
            S.dma("sp", lambda q, e=e: q.dma_start(out=XM[e * 128:(e + 1) * 128, :], in_=zt[:]), reads=["zt"], writes=[])
        ci = 0
        for tt in range(4):
            ti = tt % 2
            S.dma("sp", lambda q: q.dma_start(out=at[ti][:], in_=AT_T[:, tt * 512:(tt + 1) * 512].rearrange("(k p) t -> p k t", p=128)),
                  writes=[("at", ti)])
            S.dma("sp", lambda q: q.dma_start(out=yt[ti][:], in_=YN_T[:, tt * 512:(tt + 1) * 512].rearrange("(k p) t -> p k t", p=128)),
                  writes=["yt"])
            for cu in range(4):
                ui = cu % 2
                wa, wakey = load_w_bf(wr5, w_bra[:, cu * 512:(cu + 1) * 512], 8)
                ws, wskey = load_w_bf(wr5, w_brs[:, cu * 512:(cu + 1) * 512], 16)
                S.dma("sp", lambda q: q.dma_start(
                    out=gag[ui][:], in_=G_T[cu * 512:(cu + 1) * 512, tt * 512:(tt + 1) * 512].rearrange("(c p) t -> p c t", p=128)),
                    writes=[("gag", ui)])
                S.dma("sp", lambda q: q.dma_start(
                    out=gsg[ui][:], in_=G_T[2048 + cu * 512:2048 + (cu + 1) * 512, tt * 512:(tt + 1) * 512].rearrange("(c p) t -> p c t", p=128)),
                    writes=[("gsg", ui)])
                for cc in range(4):
                    mi = ci % 2
                    ci += 1
                    ba, bs = (0, 1) if mi == 0 else (2, 3)
                    for k in range(8):
                        S.op("pe", lambda k=k, wa=wa, ba=ba: nc.tensor.matmul(bank(ba), lhsT=wa[:, k, cc * 128:(cc + 1) * 128], rhs=at[ti][:, k, :],
                                                                       start=(k == 0), stop=(k == 7)),
                             reads=[wakey, ("at", ti)], writes=[PK(ba)])
                    for k in range(16):
                        S.op("pe", lambda k=k, ws=ws, bs=bs: nc.tensor.matmul(bank(bs), lhsT=ws[:, k, cc * 128:(cc + 1) * 128], rhs=yt[ti][:, k, :],
                                                                       start=(k == 0), stop=(k == 15)),
                             reads=[wskey, "yt"], writes=[PK(bs)])
                    S.op("dve", lambda ba=ba, mi=mi: nc.vector.tensor_tensor(out=m1[mi][:], in0=bank(ba), in1=gag[ui][:, cc, :], op=ALU.mult),
                         reads=[PK(ba), ("gag", ui)], writes=[("m1", mi)])
                    S.op("dve", lambda bs=bs, mi=mi: nc.vector.tensor_tensor(out=m2[mi][:], in0=bank(bs), in1=gsg[ui][:, cc, :], op=ALU.mult),
                         reads=[PK(bs), ("gsg", ui)], writes=[("m2", mi)])
                    S.op("dve", lambda mi=mi: nc.vector.tensor_tensor(out=mst[ti][:, cu * 4 + cc, :], in0=m1[mi][:], in1=m2[mi][:], op=ALU.add),
                         reads=[("m1", mi), ("m2", mi)], writes=[("mst", ti)])
            S.dma("sp", lambda q: q.dma_start(out=M_T[:, tt * 512:(tt + 1) * 512].rearrange("(k p) t -> p k t", p=128), in_=mst[ti][:]),
                  reads=[("mst", ti)], writes=[])
        S.barrier()
    if stop_after == "B5a":
        es.close()
        return nc

    with ExitStack() as ph:
        lnr = make_ln(ph, "ln1", 2)
        wo = sb("wo", [128, KC, D], BF16, ph)
        for un in range(4):
            S.dma("pool", lambda q, un=un: q.dma_start(out=wo[:, :, un * 512:(un + 1) * 512],
                                                      in_=w_o[:, un * 512:(un + 1) * 512].rearrange("(k p) n -> p k n", p=128)),
                  writes=[("wo", un)])
        wrr = sb("wrr", [128, KC, 72], F32, ph)
        S.dma("sp", lambda q: q.dma_start(out=wrr[:], in_=w_r.rearrange("(k p) n -> p k n", p=128)), writes=["wrr"])
        hb = [sb("hb%d" % i, [128, D], BF16, ph) for i in range(2)]
        S.op("dve", lambda: nc.vector.memset(cnt_bc[:], 0.0), writes=["cnt"])
        mt = [sb("mt%d" % i, [128, KC, 128], BF16, ph) for i in range(2)]
        uc = [sb("uc%d" % i, [128, D], F32, ph) for i in range(2)]
        hs = [sb("hs%d" % i, [128, D], F32, ph) for i in range(2)]
        h1T = sb("h1T", [128, KC, 128], F32, ph)
        r = {n: sb("r_" + n, [128, w], F32, ph) for n, w in
             [("lg", 72), ("gmax", 2), ("ohg", 8), ("eg", 8), ("sume", 2), ("t3", 64), ("les", 8), ("m1", 2), ("oh1", 8),
              ("les2", 8), ("m2", 2), ("oh2", 8), ("w", 4), ("A1", 64), ("A2", 64), ("A", 64), ("posb", 64), ("t64", 64), ("sl", 2)]}
        for c in range(16):
            i = c % 2
            S.dma("sp", lambda q: q.dma_start(out=mt[i][:], in_=M_T[:, c * 128:(c + 1) * 128].rearrange("(k p) t -> p k t", p=128)),
                  writes=[("mt", i)])
            S.dma("sp", lambda q: q.dma_start(out=uc[i][:], in_=U[c * 128:(c + 1) * 128, :]), writes=[("uc", i)])
            for ou in range(4):
                for k in range(KC):
                    S.op("pe", lambda k=k, ou=ou: nc.tensor.matmul(bank(4 + ou), lhsT=mt[i][:, k, :], rhs=wo[:, k, ou * 512:(ou + 1) * 512],
                                                                   start=(k == 0), stop=(k == KC - 1)),
                         reads=[("mt", i), ("wo", ou)], writes=[PK(4 + ou)])
            S.op("dve", lambda: nc.vector.scalar_tensor_tensor(out=hs[i][:], in0=uc[i][:], scalar=ALPHA, in1=PS[:, 2048:4096],
                                                               op0=ALU.mult, op1=ALU.add),
                 reads=[("uc", i)] + [PK(4 + ou) for ou in range(4)], writes=[("hs", i)])
            layer_norm(lnr, i, hs[i][:], ("hs", i), hs[i][:], ("hs", i))
            S.dma("sp", lambda q: q.dma_start(out=H1[c * 128:(c + 1) * 128, :], in_=hs[i][:]), reads=[("hs", i)], writes=[])
            S.op("act", lambda: nc.scalar.copy(out=hb[i][:], in_=hs[i][:]), reads=[("hs", i)], writes=[("hb", i)])
            for k in range(KC):
                S.op("pe", lambda k=k: nc.tensor.transpose(out=PS[:, k * 128:(k + 1) * 128], in_=hs[i][:, k * 128:(k + 1) * 128],
                                                           identity=ident_f),
                     reads=[("hs", i), "cst"], writes=[PK(k // 4)])
            S.op("act", lambda: nc.scalar.copy(out=h1T[:], in_=PS[:, 0:2048].rearrange("p (k t) -> p k t", k=KC)),
                 reads=[PK(b) for b in range(4)], writes=["h1T"])
            for k in range(KC):
                S.op("pe", lambda k=k: nc.tensor.matmul(PS[:, 0:72], lhsT=h1T[:, k, :], rhs=wrr[:, k, :], start=(k == 0), stop=(k == KC - 1)),
                     reads=["h1T", "wrr"], writes=[PK(0)])
            V = nc.vector
            lg, gmax, ohg, eg, sume, t3, les = r["lg"], r["gmax"], r["ohg"], r["eg"], r["sume"], r["t3"], r["les"]
            S.op("dve", lambda: V.tensor_tensor(out=lg[:], in0=PS[:, 0:72], in1=brt_bc, op=ALU.add), reads=[PK(0), "sm"], writes=["lg"])
            S.op("dve", lambda: V.tensor_reduce(out=gmax[:, 0:1], in_=lg[:, 0:8], axis=AX.X, op=ALU.max), reads=["lg"], writes=["gmax"])
            S.op("dve", lambda: V.tensor_scalar(out=ohg[:], in0=lg[:, 0:8], scalar1=gmax[:, 0:1], scalar2=None, op0=ALU.is_equal),
                 reads=["lg", "gmax"], writes=["ohg"])
            S.op("dve", lambda: V.tensor_scalar(out=gmax[:, 1:2], in0=gmax[:, 0:1], scalar1=-1.0, scalar2=None, op0=ALU.mult),
                 reads=["gmax"], writes=["ngmax"])
            S.op("act", lambda: nc.scalar.activation(out=eg[:], in_=lg[:, 0:8], func=AF.Exp, bias=gmax[:, 1:2], scale=1.0,
                                                     accum_out=sume[:, 0:1]),
                 reads=["lg", "ngmax"], writes=["eg", "sume"])
            S.op("dve", lambda: V.reciprocal(out=sume[:, 1:2], in_=sume[:, 0:1]), reads=["sume"], writes=["ptop"])
            S.op("dve", lambda: V.tensor_tensor(out=t3[:].rearrange("p (e g) -> p e g", g=8),
                                                in0=lg[:, 8:72].rearrange("p (g e) -> p e g", e=8),
                                                in1=ohg[:].rearrange("p (o g) -> p o g", o=1).to_broadcast([128, 8, 8]), op=ALU.mult),
                 reads=["lg", "ohg"], writes=["t3"])
            S.op("dve", lambda: V.tensor_reduce(out=les[:], in_=t3[:].rearrange("p (e g) -> p e g", g=8), axis=AX.X, op=ALU.add),
                 reads=["t3"], writes=["les"])
            S.op("dve", lambda: V.tensor_reduce(out=r["m1"][:, 0:1], in_=les[:], axis=AX.X, op=ALU.max), reads=["les"], writes=["m1"])
            S.op("dve", lambda: V.tensor_scalar(out=r["oh1"][:], in0=les[:], scalar1=r["m1"][:, 0:1], scalar2=None, op0=ALU.is_equal),
                 reads=["les", "m1"], writes=["oh1"])
            S.op("dve", lambda: V.scalar_tensor_tensor(out=r["les2"][:], in0=r["oh1"][:], scalar=-1e30, in1=les[:], op0=ALU.mult, op1=ALU.add),
                 reads=["oh1", "les"], writes=["les2"])
            S.op("dve", lambda: V.tensor_reduce(out=r["m2"][:, 0:1], in_=r["les2"][:], axis=AX.X, op=ALU.max), reads=["les2"], writes=["m2"])
            S.op("dve", lambda: V.tensor_scalar(out=r["oh2"][:], in0=r["les2"][:], scalar1=r["m2"][:, 0:1], scalar2=None, op0=ALU.is_equal),
                 reads=["les2", "m2"], writes=["oh2"])
            S.op("dve", lambda: V.tensor_tensor(out=r["w"][:, 0:1], in0=r["m1"][:, 0:1], in1=r["m2"][:, 0:1], op=ALU.subtract),
                 reads=["m1", "m2"], writes=["w0"])
            S.op("act", lambda: nc.scalar.activation(out=r["w"][:, 1:2], in_=r["w"][:, 0:1], func=AF.Sigmoid), reads=["w0"], writes=["w1"])
            S.op("dve", lambda: V.tensor_scalar(out=r["w"][:, 2:3], in0=r["w"][:, 1:2], scalar1=-1.0, scalar2=1.0, op0=ALU.mult, op1=ALU.add),
                 reads=["w1"], writes=["w2"])
            S.op("dve", lambda: V.tensor_scalar(out=rtw[:, c, :], in0=r["w"][:, 1:3], scalar1=sume[:, 1:2], scalar2=None, op0=ALU.mult),
                 reads=["w1", "w2", "ptop"], writes=[("rtw", c)])
            for nm, oh in (("A1", "oh1"), ("A2", "oh2")):
                S.op("dve", lambda nm=nm, oh=oh: V.tensor_tensor(
                    out=r[nm][:].rearrange("p (g e) -> p g e", e=8),
                    in0=ohg[:].rearrange("p (g o) -> p g o", o=1).to_broadcast([128, 8, 8]),
                    in1=r[oh][:].rearrange("p (o e) -> p o e", o=1).to_broadcast([128, 8, 8]), op=ALU.mult),
                    reads=["ohg", oh], writes=[nm])
            S.op("dve", lambda: V.tensor_tensor(out=r["A"][:], in0=r["A1"][:], in1=r["A2"][:], op=ALU.add), reads=["A1", "A2"], writes=["A"])
            S.op("pe", lambda: nc.tensor.matmul(PS[:, 512:576], lhsT=triS, rhs=r["A"][:], start=True, stop=True),
                 reads=["A", "cst"], writes=[PK(1)])
            S.op("pe", lambda: nc.tensor.matmul(PS[:, 576:640], lhsT=ones_f, rhs=r["A"][:], start=True, stop=True),
                 reads=["A", "cst"], writes=[PK(1)])
            S.op("dve", lambda: V.tensor_tensor(out=r["posb"][:], in0=PS[:, 512:576], in1=cnt_bc[:], op=ALU.add),
                 reads=[PK(1), "cnt"], writes=["posb"])
            S.op("dve", lambda: V.tensor_tensor(out=r["posb"][:], in0=r["posb"][:], in1=ebase_bc, op=ALU.add), reads=["posb", "sm"], writes=["posb"])
            S.op("dve", lambda: V.tensor_tensor(out=cnt_bc[:], in0=cnt_bc[:], in1=PS[:, 576:640], op=ALU.add),
                 reads=["cnt", PK(1), "posb"], writes=["cnt"])
            for kk, nm in enumerate(("A1", "A2")):
                S.op("dve", lambda nm=nm: V.tensor_tensor(out=r["t64"][:], in0=r[nm][:], in1=r["posb"][:], op=ALU.mult),
                     reads=[nm, "posb"], writes=["t64"])
                S.op("dve", lambda kk=kk: V.tensor_reduce(out=r["sl"][:, kk:kk + 1], in_=r["t64"][:], axis=AX.X, op=ALU.add),
                     reads=["t64"], writes=[("sl", kk)])
                S.op("dve", lambda kk=kk: V.tensor_copy(out=rts[:, c, kk:kk + 1], in_=r["sl"][:, kk:kk + 1]),
                     reads=[("sl", kk)], writes=[("rts", c, kk)])
                S.dma("pool", lambda q, kk=kk: q.indirect_dma_start(
                    out=XM[:, :], out_offset=bass.IndirectOffsetOnAxis(ap=rts[:, c, kk:kk + 1], axis=0), in_=hb[i][:], in_offset=None),
                    reads=[("rts", c, kk), ("hb", i)], writes=[])
        S.barrier()
    if stop_after == "B5b":
        es.close()
        return nc

    with ExitStack() as ph:
        xe = [sb("xe%d" % i, [128, D], BF16, ph) for i in range(2)]
        xeT = [sb("xeT%d" % i, [128, KC, 128], BF16, ph) for i in range(2)]
        wg = [sb("wg%d" % i, [128, KC, 512], BF16, ph) for i in range(2)]
        wu = [sb("wu%d" % i, [128, KC, 512], BF16, ph) for i in range(2)]
        wd = [sb("wd%d" % i, [128, 4, D], BF16, ph) for i in range(2)]
        sg = [sb("sg%d" % i, [128, 512], F32, ph) for i in range(2)]
        hh_ = [sb("hh%d" % i, [128, 512], BF16, ph) for i in range(2)]
        hT = [sb("hT%d" % i, [128, 4, 128], BF16, ph) for i in range(2)]
        ye = [sb("ye%d" % i, [128, D], F32, ph) for i in range(2)]
        yrot = 0
        for e in range(64):
            i = e % 2
            S.dma("pool", lambda q: q.dma_start(out=wg[i][:], in_=w_gate[e].rearrange("(k p) f -> p k f", p=128)), writes=[("wg", i)])
            S.dma("pool", lambda q: q.dma_start(out=wu[i][:], in_=w_up[e].rearrange("(k p) f -> p k f", p=128)), writes=[("wu", i)])
            for hf in range(2):
                S.dma("pool", lambda q, hf=hf: q.dma_start(out=wd[i][:, :, hf * 1024:(hf + 1) * 1024],
                                                          in_=w_down[e][:, hf * 1024:(hf + 1) * 1024].rearrange("(c p) o -> p c o", p=128)),
                      writes=[("wd", i, hf)])
            S.dma("sp", lambda q: q.dma_start(out=xe[i][:], in_=XM[e * 128:(e + 1) * 128, :]), writes=[("xe", i)])
            pv = bank_bf(0)
            for k in range(KC):
                S.op("pe", lambda k=k, pv=pv: nc.tensor.transpose(out=pv[:, k * 128:(k + 1) * 128], in_=xe[i][:, k * 128:(k + 1) * 128],
                                                                  identity=ident_b[:]),
                     reads=[("xe", i), "ident_b"], writes=[PK(0), PK(1)])
            S.op("act", lambda pv=pv: nc.scalar.copy(out=xeT[i][:], in_=pv.rearrange("p (k t) -> p k t", k=KC)),
                 reads=[PK(0), PK(1)], writes=[("xeT", i)])
            for k in range(KC):
                S.op("pe", lambda k=k: nc.tensor.matmul(bank(2), lhsT=xeT[i][:, k, :], rhs=wg[i][:, k, :], start=(k == 0), stop=(k == KC - 1)),
                     reads=[("wg", i), ("xeT", i)], writes=[PK(2)])
            for k in range(KC):
                S.op("pe", lambda k=k: nc.tensor.matmul(bank(3), lhsT=xeT[i][:, k, :], rhs=wu[i][:, k, :], start=(k == 0), stop=(k == KC - 1)),
                     reads=[("wu", i), ("xeT", i)], writes=[PK(3)])
            S.op("act", lambda: nc.scalar.activation(out=sg[i][:], in_=bank(2), func=AF.Silu), reads=[PK(2)], writes=[("sg", i)])
            S.op("dve", lambda: nc.vector.tensor_tensor(out=hh_[i][:], in0=sg[i][:], in1=bank(3), op=ALU.mult),
                 reads=[("sg", i), PK(3)], writes=[("hh", i)])
            pv4 = bank_bf(4, 1)
            for fc in range(4):
                S.op("pe", lambda fc=fc, pv4=pv4: nc.tensor.transpose(out=pv4[:, fc * 128:(fc + 1) * 128], in_=hh_[i][:, fc * 128:(fc + 1) * 128],
                                                                      identity=ident_b[:]),
                     reads=[("hh", i), "ident_b"], writes=[PK(4)])
            S.op("act", lambda pv4=pv4: nc.scalar.copy(out=hT[i][:], in_=pv4[:, 0:512].rearrange("p (c t) -> p c t", c=4)),
                 reads=[PK(4)], writes=[("hT", i)])
            for ou in range(4):
                yb = 5 + yrot % 3
                yrot += 1
                for fc in range(4):
                    S.op("pe", lambda fc=fc, yb=yb, ou=ou: nc.tensor.matmul(bank(yb), lhsT=hT[i][:, fc, :], rhs=wd[i][:, fc, ou * 512:(ou + 1) * 512],
                                                                            start=(fc == 0), stop=(fc == 3)),
                         reads=[("hT", i), ("wd", i, ou // 2)], writes=[PK(yb)])
                if ou % 2 == 0:
                    S.op("act", lambda yb=yb, ou=ou: nc.scalar.copy(out=ye[i][:, ou * 512:(ou + 1) * 512], in_=bank(yb)),
                         reads=[PK(yb)], writes=[("ye", i, ou)])
                else:
                    S.op("dve", lambda yb=yb, ou=ou: nc.vector.tensor_copy(out=ye[i][:, ou * 512:(ou + 1) * 512], in_=bank(yb)),
                         reads=[PK(yb)], writes=[("ye", i, ou)])
            S.dma("sp", lambda q: q.dma_start(out=YS[e * 128:(e + 1) * 128, :], in_=ye[i][:]), reads=[("ye", i, ou) for ou in range(4)],
                  writes=[])
        S.barrier()

    with ExitStack() as ph:
        lnr = make_ln(ph, "ln2", 4)
        g1 = [sb("g1_%d" % i, [128, D], F32, ph) for i in range(2)]
        g2 = [sb("g2_%d" % i, [128, D], F32, ph) for i in range(2)]
        hc = [sb("hc%d" % i, [128, D], F32, ph) for i in range(2)]
        for c in range(16):
            i = c % 2
            S.dma("pool", lambda q: q.indirect_dma_start(out=g1[i][:], out_offset=None, in_=YS[:, :],
                                                         in_offset=bass.IndirectOffsetOnAxis(ap=rts[:, c, 0:1], axis=0)),
                  writes=[("g1", i)])
            S.dma("pool", lambda q: q.indirect_dma_start(out=g2[i][:], out_offset=None, in_=YS[:, :],
                                                         in_offset=bass.IndirectOffsetOnAxis(ap=rts[:, c, 1:2], axis=0)),
                  writes=[("g2", i)])
            S.dma("sp", lambda q: q.dma_start(out=hc[i][:], in_=H1[c * 128:(c + 1) * 128, :]), writes=[("hc", i)])
            S.op("dve", lambda: nc.vector.tensor_scalar(out=g1[i][:], in0=g1[i][:], scalar1=rtw[:, c, 0:1], scalar2=None, op0=ALU.mult),
                 reads=[("g1", i)], writes=[("g1", i)])
            S.op("dve", lambda: nc.vector.scalar_tensor_tensor(out=g1[i][:], in0=g2[i][:], scalar=rtw[:, c, 1:2], in1=g1[i][:],
                                                               op0=ALU.mult, op1=ALU.add),
                 reads=[("g1", i), ("g2", i)], writes=[("g1", i)])
            S.op("dve", lambda: nc.vector.scalar_tensor_tensor(out=hc[i][:], in0=hc[i][:], scalar=ALPHA, in1=g1[i][:],
                                                               op0=ALU.mult, op1=ALU.add),
                 reads=[("g1", i), ("hc", i)], writes=[("hc", i)])
            layer_norm(lnr, i, hc[i][:], ("hc", i), hc[i][:], ("hc", i))
            S.dma("sp", lambda q: q.dma_start(out=out[c * 128:(c + 1) * 128, :], in_=hc[i][:]), reads=[("hc", i)], writes=[])
        S.barrier()
    es.close()
    return nc


def host_inputs(inputs):
    x = np.asarray(inputs["x"], np.float32)
    meta = np.asarray(inputs["meta_tokens"], np.float32)
    f = lambda k: np.asarray(inputs[k], np.float32)
    rep = lambda v: np.ascontiguousarray(np.broadcast_to(v.reshape(1, -1), (128, v.size)))
    metablk = np.concatenate([np.zeros((112, D), np.float32), meta], 0)
    p = np.arange(128)
    ident = (p[:, None] == p[None, :]).astype(np.float32)
    tri_le = (p[:, None] <= p[None, :]).astype(np.float32)
    triU = (p[:, None] > p[None, :]).astype(np.float32)
    ones = np.ones((128, 128), np.float32)
    maskneg = np.where(p[None, :] >= p[:, None], 0.0, -30000.0).astype(np.float32)
    triS = (p[:, None] < p[None, :]).astype(np.float32)
    mcur = (p[:, None] <= p[None, :]).astype(np.float32)
    mprev = (p[:, None] > p[None, :]).astype(np.float32)
    consts = np.concatenate([ident, tri_le, triU, ones, maskneg, triS, mcur, mprev], 1)
    mmeta = np.zeros((128, 128), np.float32)
    mmeta[112:, :] = 1.0
    lnp = np.concatenate([rep(f("ln_emb_g")), rep(f("ln_emb_b")), rep(f("ln1_g")[0]), rep(f("ln1_b")[0]),
                          rep(f("ln2_g")[0]), rep(f("ln2_b")[0])], 1)
    cwm = f("conv_w")[0]
    convw = np.ascontiguousarray(cwm.reshape(4, 24, 128).transpose(2, 1, 0).reshape(128, 96))
    convb = np.ascontiguousarray(f("conv_b")[0].reshape(24, 128).T)
    ebase = (np.arange(64) * 128).astype(np.float32)
    small = np.concatenate([rep(f("dt_bias")[0]), rep(f("a_log")[0]), rep(f("d_skip")[0]), rep(f("sinks")[0]),
                            rep(f("b_router_group")[0]), rep(f("b_router_expert")[0]), rep(ebase)], 1)
    w_r = np.ascontiguousarray(np.concatenate([f("w_router_group")[0], f("w_router_expert")[0]], 1))
    shared = {
        "consts": consts, "lnp": lnp, "gnorm": rep(f("ssd_norm_g")[0]), "w_in": f("w_in")[0], "convw": convw,
        "convb": convb, "small": small, "w_bra": f("w_br_attn")[0], "w_brs": f("w_br_ssd")[0], "w_o": f("w_o")[0],
        "w_r": w_r, "w_gate": f("w_gate")[0], "w_up": f("w_up")[0], "w_down": f("w_down")[0],
    }
    maps = []
    for c in range(8):
        b, j = c // 4, c % 4
        if j == 0:
            halo = metablk
            cmask = np.concatenate([np.zeros(112, np.float32), np.ones(16, np.float32)])
            dmask = cmask.copy()
        else:
            halo = x[b, OWN * j - 128:OWN * j]
            cmask = np.ones(128, np.float32)
            dmask = np.zeros(128, np.float32)
        xb = np.concatenate([metablk, halo, x[b, OWN * j:OWN * (j + 1)]], 0)
        sel = np.zeros(8, np.float32)
        sel[4 * b:4 * b + j] = 1.0
        hmask = np.concatenate([rep(cmask), dmask.reshape(128, 1), rep(sel)], 1)
        mfirst = np.concatenate([np.zeros((128, 128), np.float32) if j == 0 else mprev, mmeta], 1)
        m = dict(shared)
        m.update({"xb": np.ascontiguousarray(xb), "hmask": np.ascontiguousarray(hmask), "mfirst": mfirst})
        maps.append(m)
    return maps


def kernel(**inputs):
    nc = build()
    maps = host_inputs(inputs)
    res = run_bass_kernel_spmd(nc, maps, core_ids=list(range(8)))
    o = np.stack([np.asarray(r["out"], np.float32) for r in res.results], 0)
    return np.ascontiguousarray(o.reshape(2, 4 * OWN, D))
```
